# Optimizing a Trainium2 kernel written in Bass

```python
import jax, jax.numpy as jnp
from jax import lax
import numpy as np

D_MODEL = 4096
BATCH = 2
SEQ = 8192
DEPTH = 2

N_MIXERS = 2
N_CONV_LAYERS = (DEPTH + 1) // 2
N_MLSTM_LAYERS = DEPTH // 2

E_CONV = D_MODEL
CONV_WIDTH = 31

N_HEADS = 8
DV = D_MODEL // N_HEADS
DQK = DV // 2
QK_CONV_WIDTH = 4
CHUNK = 128
HQK = N_HEADS * DQK
HV = N_HEADS * DV
MLSTM_PROJ = 2 * HQK + HV + 2 * N_HEADS + HV + HV

NORM_EPS = 1e-6
LN_EPS = 1e-5

kernel_name = "hybrid_conformer_conv_mlstm_sandwich"


def rms_norm(x, g, eps=NORM_EPS):
    xf = x.astype(jnp.float32)
    y = xf * lax.rsqrt(jnp.mean(xf * xf, axis=-1, keepdims=True) + eps)
    return (y * g.astype(jnp.float32)).astype(x.dtype)


def layer_norm(x, g, b, eps=LN_EPS):
    xf = x.astype(jnp.float32)
    mu = jnp.mean(xf, axis=-1, keepdims=True)
    xc = xf - mu
    var = jnp.mean(xc * xc, axis=-1, keepdims=True)
    y = xc * lax.rsqrt(var + eps) * g.astype(jnp.float32) + b.astype(jnp.float32)
    return y.astype(x.dtype)


def causal_depthwise_conv(x, w, b):
    k, c = w.shape
    y = lax.conv_general_dilated(
        x, w[:, None, :].astype(x.dtype), window_strides=(1,), padding=[(k - 1, 0)],
        dimension_numbers=("NWC", "WIO", "NWC"), feature_group_count=c)
    return y + b.astype(x.dtype)


def conv_mixer(h, w_in, dw_w, dw_b, ln_g, ln_b, w_out):
    a, gl, z = jnp.split(h @ w_in, 3, axis=-1)
    u = a * jax.nn.sigmoid(gl)
    u = causal_depthwise_conv(u, dw_w, dw_b)
    u = layer_norm(u, ln_g, ln_b)
    u = jax.nn.silu(u) * jax.nn.silu(z)
    return u @ w_out


def mlstm_chunkwise(q, k, v, ig, fg):
    bsz, s, nh, dk = q.shape
    dv = v.shape[-1]
    nc = s // CHUNK
    out_dtype = v.dtype

    def to_chunks(t):
        t = t.astype(jnp.float32).reshape((bsz, nc, CHUNK) + t.shape[2:])
        return jnp.moveaxis(jnp.moveaxis(t, 1, 0), 3, 2)

    qc = to_chunks(q) * (dk ** -0.5)
    kc = to_chunks(k)
    vc = to_chunks(v)
    icg = to_chunks(ig)
    lfg = to_chunks(jax.nn.log_sigmoid(fg.astype(jnp.float32)))
    mask = jnp.tril(jnp.ones((CHUNK, CHUNK), dtype=bool))

    def step(carry, inp):
        c_st, n_st, m_st = carry
        qb, kb, vb, ib, lf = inp
        b = lax.cumsum(lf, axis=2)
        g = b[..., -1]
        dmat = b[..., :, None] - b[..., None, :] + ib[..., None, :]
        dmat = jnp.where(mask, dmat, -jnp.inf)
        m_inter = b + m_st[..., None]
        m_t = jnp.maximum(m_inter, jnp.max(dmat, axis=-1))
        scores = jnp.einsum("bhtd,bhsd->bhts", qb, kb) * jnp.exp(dmat - m_t[..., None])
        decay = jnp.exp(m_inter - m_t)
        num = (jnp.einsum("bhts,bhsv->bhtv", scores, vb)
               + decay[..., None] * jnp.einsum("bhtd,bhdv->bhtv", qb, c_st))
        den = jnp.sum(scores, axis=-1) + decay * jnp.einsum("bhtd,bhd->bht", qb, n_st)
        hb = num / jnp.maximum(jnp.abs(den), jnp.exp(-m_t))[..., None]
        a = g[..., None] - b + ib
        m_new = jnp.maximum(g + m_st, jnp.max(a, axis=-1))
        wa = jnp.exp(a - m_new[..., None])
        dc = jnp.exp(g + m_st - m_new)
        c_st = dc[..., None, None] * c_st + jnp.einsum("bhs,bhsd,bhsv->bhdv", wa, kb, vb)
        n_st = dc[..., None] * n_st + jnp.einsum("bhs,bhsd->bhd", wa, kb)
        return (c_st, n_st, m_new), hb

    init = (jnp.zeros((bsz, nh, dk, dv), jnp.float32),
            jnp.zeros((bsz, nh, dk), jnp.float32),
            jnp.zeros((bsz, nh), jnp.float32))
    _, h = lax.scan(step, init, (qc, kc, vc, icg, lfg))
    h = jnp.moveaxis(jnp.moveaxis(h, 2, 3), 0, 1).reshape(bsz, s, nh, dv)
    return h.astype(out_dtype)


def mlstm_mixer(h, w_in, qk_conv_w, qk_conv_b, gate_b, head_norm_g, w_out):
    bsz, s, _ = h.shape
    u = h @ w_in
    qk, v, gates, o, z = jnp.split(
        u, [2 * HQK, 2 * HQK + HV, 2 * HQK + HV + 2 * N_HEADS, 2 * HQK + 2 * HV + 2 * N_HEADS],
        axis=-1)
    qk = jax.nn.silu(causal_depthwise_conv(qk, qk_conv_w, qk_conv_b))
    q, k = jnp.split(qk, 2, axis=-1)
    q = q.reshape(bsz, s, N_HEADS, DQK)
    k = k.reshape(bsz, s, N_HEADS, DQK)
    v = v.reshape(bsz, s, N_HEADS, DV)
    gates = gates.astype(jnp.float32) + gate_b.astype(jnp.float32)
    ig, fg = jnp.split(gates, 2, axis=-1)
    hh = mlstm_chunkwise(q, k, v, ig, fg)
    hh = rms_norm(hh, head_norm_g).reshape(bsz, s, HV)
    hh = hh * jax.nn.sigmoid(o) * jax.nn.silu(z)
    return hh @ w_out


def setup_inputs(seed: int = 0) -> dict:
    key = jax.random.key(seed)
    ks = jax.random.split(key, 20)
    f32 = jnp.float32
    nrm = lambda k, shape, scale: jax.random.normal(k, shape, f32) * scale
    na, nb = N_CONV_LAYERS, N_MLSTM_LAYERS
    f_bias = jnp.linspace(3.0, 6.0, N_HEADS, dtype=f32)
    gate_b = jnp.concatenate(
        [nrm(ks[13], (nb, N_HEADS), 0.1),
         f_bias[None, :] + nrm(ks[14], (nb, N_HEADS), 0.1)], axis=-1)
    return {
        "x": nrm(ks[0], (BATCH, SEQ, D_MODEL), 1.0),
        "pre_norm_g": 1.0 + nrm(ks[1], (DEPTH, D_MODEL), 0.02),
        "post_norm_g": 1.0 + nrm(ks[2], (DEPTH, D_MODEL), 0.02),
        "conv_w_in": nrm(ks[3], (na, D_MODEL, 3 * E_CONV), D_MODEL ** -0.5),
        "conv_dw_w": nrm(ks[4], (na, CONV_WIDTH, E_CONV), CONV_WIDTH ** -0.5),
        "conv_dw_b": nrm(ks[5], (na, E_CONV), 0.01),
        "conv_ln_g": 1.0 + nrm(ks[6], (na, E_CONV), 0.02),
        "conv_ln_b": nrm(ks[7], (na, E_CONV), 0.01),
        "conv_w_out": nrm(ks[8], (na, E_CONV, D_MODEL), E_CONV ** -0.5),
        "mlstm_w_in": nrm(ks[9], (nb, D_MODEL, MLSTM_PROJ), D_MODEL ** -0.5),
        "mlstm_qk_conv_w": nrm(ks[10], (nb, QK_CONV_WIDTH, 2 * HQK), QK_CONV_WIDTH ** -0.5),
        "mlstm_qk_conv_b": nrm(ks[11], (nb, 2 * HQK), 0.01),
        "mlstm_gate_b": gate_b,
        "mlstm_head_norm_g": 1.0 + nrm(ks[12], (nb, N_HEADS, DV), 0.02),
        "mlstm_w_out": nrm(ks[15], (nb, HV, D_MODEL), HV ** -0.5),
    }


def reference(x, pre_norm_g, post_norm_g, conv_w_in, conv_dw_w, conv_dw_b, conv_ln_g,
              conv_ln_b, conv_w_out, mlstm_w_in, mlstm_qk_conv_w, mlstm_qk_conv_b,
              mlstm_gate_b, mlstm_head_norm_g, mlstm_w_out):
    for i in range(DEPTH):
        h = rms_norm(x, pre_norm_g[i])
        j = i // N_MIXERS
        if i % N_MIXERS == 0:
            y = conv_mixer(h, conv_w_in[j], conv_dw_w[j], conv_dw_b[j], conv_ln_g[j],
                           conv_ln_b[j], conv_w_out[j])
        else:
            y = mlstm_mixer(h, mlstm_w_in[j], mlstm_qk_conv_w[j], mlstm_qk_conv_b[j],
                            mlstm_gate_b[j], mlstm_head_norm_g[j], mlstm_w_out[j])
        x = x + rms_norm(y, post_norm_g[i])
    return x
```

```python
import numpy as np
import ml_dtypes
from contextlib import ExitStack
import concourse.bass as bass
import concourse.mybir as mybir
from concourse.bass_utils import run_bass_kernel_spmd

F32 = mybir.dt.float32
BF16 = mybir.dt.bfloat16
AF = mybir.ActivationFunctionType
ALU = mybir.AluOpType

D = 4096
KC = 32
NSEG = 2048
HALO = 128
NTOK = NSEG + HALO
NH = 8
WC = 256
NWS = 3
EPS = 1e-6
LN_EPS = 1e-5


class Sched:
    def __init__(self, nc, es):
        self.nc = nc
        self.es = es
        self.engs = {"pe": nc.tensor, "act": nc.scalar, "dve": nc.vector, "pool": nc.gpsimd, "sp": nc.sync}
        self.sems = {}
        self.cnt = {}
        self.waited = {e: {} for e in self.engs}
        self.lastw = {}
        self.readers = {}

    def sem(self, name):
        if name not in self.sems:
            self.sems[name] = self.es.enter_context(self.nc.semaphore(name))
            self.cnt[name] = 0
        return self.sems[name]

    def _deps(self, eng, reads, writes):
        me = "e_" + eng
        deps = {}

        def add(tok, raw):
            s, v = tok
            if s == me and (eng == "pe" or not raw):
                return
            if deps.get(s, 0) < v:
                deps[s] = v

        for k in reads:
            for s, v in self.lastw.get(k, {}).items():
                add((s, v), True)
        for k in writes:
            for s, v in self.lastw.get(k, {}).items():
                add((s, v), False)
            for s, v in self.readers.get(k, {}).items():
                add((s, v), False)
        E = self.engs[eng]
        w = self.waited[eng]
        for s, v in deps.items():
            if w.get(s, 0) < v:
                E.wait_ge(self.sems[s], v)
                w[s] = v

    def _record(self, tok, reads, writes):
        for k in writes:
            w = self.lastw.setdefault(k, {})
            if w.get(tok[0], 0) < tok[1]:
                w[tok[0]] = tok[1]
            self.readers[k] = {}
        for k in reads:
            r = self.readers.setdefault(k, {})
            if r.get(tok[0], 0) < tok[1]:
                r[tok[0]] = tok[1]

    def op(self, eng, fn, reads=(), writes=(), inc=True):
        self._deps(eng, reads, writes)
        ins = fn()
        me = "e_" + eng
        self.sem(me)
        if inc:
            ins.then_inc(self.sems[me], 1)
            self.cnt[me] += 1
            tok = (me, self.cnt[me])
        else:
            tok = (me, self.cnt[me] + 1)
        self._record(tok, reads, writes)
        return tok

    def dma(self, q, out, in_, reads=(), writes=(), dkey=None):
        self._deps(q, reads, writes)
        name = "d_" + dkey
        sem = self.sem(name)
        self.engs[q].dma_start(out=out, in_=in_).then_inc(sem, 16)
        self.cnt[name] += 16
        tok = (name, self.cnt[name])
        self._record(tok, reads, writes)
        return tok

    def wait_all(self, eng):
        E = self.engs[eng]
        for s, c in self.cnt.items():
            if c > 0 and self.waited[eng].get(s, 0) < c and s != "e_" + eng:
                E.wait_ge(self.sems[s], c)
                self.waited[eng][s] = c


class WStream:
    def __init__(self, S, slots, nslots):
        self.S = S
        self.slots = slots
        self.n = nslots
        self.chunks = []
        self.issued = 0
        self.pos = 0

    def plan(self, chunks):
        self.chunks.extend(chunks)

    def _issue(self, i):
        w, c0, ncol = self.chunks[i]
        sl = i % self.n
        src = w.rearrange("(k p) c -> p k c", p=128)[:, :, c0:c0 + ncol]
        self.S.dma("pool", self.slots[sl][:, :, 0:ncol], src, writes=[("w", sl)], dkey="w%d" % sl)

    def next(self):
        i = self.pos
        while self.issued < min(len(self.chunks), i + self.n - 1):
            self._issue(self.issued)
            self.issued += 1
        self.pos += 1
        sl = i % self.n
        return self.slots[sl], ("w", sl)


def tiles_p1():
    return [(0, 5), (5, 4), (9, 4), (13, 4)]


def nblocks(T):
    out = []
    n0 = 0
    while n0 < T:
        out.append((n0, min(512, T - n0)))
        n0 += 512
    return out


def build(phase, ntiles=4, fused=False):
    nc = bass.Bass("TRN2", target_bir_lowering=False)
    es = ExitStack()
    with es:
        S = Sched(nc, es)
        ntok1 = sum(n for _, n in tiles_p1()[:ntiles]) * 128

        def din(name, shape, dt=F32):
            return nc.dram_tensor(name, shape, dt, kind="ExternalInput").ap()

        def dmid(name, shape, dt, prod):
            if phase == 0:
                kind = "Internal"
            else:
                kind = "ExternalOutput" if phase == prod else "ExternalInput"
            return nc.dram_tensor(name, shape, dt, kind=kind).ap()

        ident_d = din("ident", [128, 128], BF16)
        tri_d = din("tri", [128, 128], F32)
        hng_d = din("hng", [128, D])
        postg_d = din("postg", [2, 128, D])
        x1s = dmid("x1s", [NTOK, D], F32, 1)
        qs = dmid("qs", [16, 128, NTOK], BF16, 1)
        ks = dmid("ks", [16, 128, NTOK], BF16, 1)
        vs = dmid("vs", [NTOK, D], BF16, 1)
        ozs = dmid("ozs", [NTOK, D], BF16, 1)
        gs = dmid("gs", [NTOK, 16], F32, 1)
        cloc = dmid("cloc", [128, NH * 2 * 512], F32, 1)
        nloc = dmid("nloc", [128, 16], F32, 1)
        gtot = dmid("gtot", [128, NH], F32, 1)
        if phase in (0, 1):
            x_d = din("x", [NTOK, D])
            w_in0 = din("w_in0", [D, 3 * D])
            w_out0 = din("w_out0", [D, D])
            w_in1 = din("w_in1", [D, 16400])
            pg_d = din("pg", [128, 2, KC])
            dww_d = din("dww", [128, KC, 31])
            cvec_d = din("cvec", [128, 3, KC])
            qkw_d = din("qkw", [128, KC, 4])
            qkb_d = din("qkb", [128, KC])
            gb_d = din("gb", [128, 16])
            hmask_d = din("hmask", [128, 1])
            ys = nc.dram_tensor("ys", [NTOK, D], F32, kind="Internal").ap()
        if phase in (0, 2):
            w_out1 = din("w_out1", [D, D])
            y2s = nc.dram_tensor("y2s", [NSEG, D], F32, kind="Internal").ap()
            out_d = nc.dram_tensor("out", [NSEG, D], F32, kind="ExternalOutput").ap()
            if phase == 2:
                call = din("call", [4, 128, NH * 2 * 512])
                nall = din("nall", [4, 128, 16])
                gtall = din("gtall", [128, 4, NH])
            cmask_d = din("cmask", [128, 4, 4])
            cvalid_d = din("cvalid", [128, 4])

        sb = lambda name, shape, dt: es.enter_context(nc.sbuf_tensor("s0_" + name, shape, dt))
        ps = lambda name, shape, dt: es.enter_context(nc.psum_tensor("ps_" + name, shape, dt))

        ident = sb("ident_s", [128, 128], BF16)
        tri = sb("tri_s", [128, 128], F32)
        onesf = sb("onesf", [128, 128], F32)
        onesb = sb("onesb", [128, 128], BF16)
        S.dma("sp", ident[:], ident_d[:, :], writes=["ident"], dkey="c")
        S.dma("sp", tri[:], tri_d[:, :], writes=["tri"], dkey="c")
        S.op("dve", lambda: nc.vector.memset(onesf[:], 1.0), writes=["onesf"])
        S.op("dve", lambda: nc.vector.memset(onesb[:], 1.0), writes=["onesb"])

        tp = [ps("tp%d" % i, [128, 1024], BF16) for i in range(2)]
        acc = [ps("acc%d" % i, [128, 512], F32) for i in range(6)]
        acc_i = [0]

        def next_acc():
            i = acc_i[0] % 6
            acc_i[0] += 1
            return acc[i], ("acc", i)

        def gate_math(gsb, gkey):
            r = {}
            bps, bk = next_acc()
            S.op("pe", lambda: nc.tensor.matmul(bps[:, 0:8], lhsT=tri[:], rhs=gsb[:, 8:16], start=True, stop=True),
                 reads=["tri", gkey], writes=[bk], inc=False)
            S.op("pe", lambda: nc.tensor.matmul(bps[:, 8:16], lhsT=onesf[:], rhs=gsb[:, 8:16], start=True, stop=True),
                 reads=["onesf", gkey], writes=[bk])
            gm = gm_t[gm_i[0] % 2]
            gmk = ("gm", gm_i[0] % 2)
            gm_i[0] += 1
            S.op("dve", lambda: nc.vector.tensor_copy(gm[:, 0:16], bps[:, 0:16]), reads=[bk], writes=[gmk])
            S.op("dve", lambda: nc.vector.tensor_tensor(out=gm[:, 24:32], in0=gsb[:, 0:8], in1=gm[:, 0:8], op=ALU.subtract),
                 reads=[gmk, gkey], writes=[gmk])
            S.op("dve", lambda: nc.vector.tensor_tensor(out=gm[:, 16:24], in0=gm[:, 24:32], in1=gm[:, 8:16], op=ALU.add),
                 reads=[gmk], writes=[gmk])
            S.op("dve", lambda: nc.vector.tensor_copy(gm[:, 32:48], gm[:, 0:16]), reads=[gmk], writes=[gmk])
            S.op("act", lambda: nc.scalar.activation(out=gm[:, 16:48], in_=gm[:, 16:48], func=AF.Exp),
                 reads=[gmk], writes=[gmk])
            return gm, gmk

        def state_update(h, kT_chunk, kkey, v_ap, vkey, gm, gmk, Cst, nst):
            bank = tp[tp_i[0] % 2]
            bkey = ("tp", tp_i[0] % 2)
            tp_i[0] += 1
            for dc in range(2):
                S.op("pe", lambda dc=dc: nc.tensor.transpose(out=bank[:, dc * 128:(dc + 1) * 128], in_=kT_chunk(dc), identity=ident[:]),
                     reads=[kkey, "ident"], writes=[bkey], inc=(dc == 1))
            ksc = ksc_t[ksc_i[0] % 2]
            ksk = ("ksc", ksc_i[0] % 2)
            ksc_i[0] += 1
            S.op("act", lambda: nc.scalar.activation(out=ksc[:, 0:256], in_=bank[:, 0:256], func=AF.Copy, scale=gm[:, 16 + h:17 + h]),
                 reads=[bkey, gmk], writes=[ksk])
            for dc in range(2):
                cps, ck = next_acc()
                S.op("pe", lambda dc=dc, cps=cps: nc.tensor.matmul(cps[:, 0:512], lhsT=ksc[:, dc * 128:(dc + 1) * 128], rhs=v_ap, start=True, stop=True),
                     reads=[ksk, vkey], writes=[ck])
                S.op("dve", lambda dc=dc, cps=cps: nc.vector.scalar_tensor_tensor(
                    out=Cst[:, h, dc, :], in0=Cst[:, h, dc, :], scalar=gm[:, 40 + h:41 + h], in1=cps[:, 0:512],
                    op0=ALU.mult, op1=ALU.add), reads=[ck, gmk, ("C", h)], writes=[("C", h)])
            nps, nk = next_acc()
            for dc in range(2):
                S.op("pe", lambda dc=dc: nc.tensor.matmul(nps[:, dc:dc + 1], lhsT=ksc[:, dc * 128:(dc + 1) * 128], rhs=onesb[:, 0:1], start=True, stop=True),
                     reads=[ksk, "onesb"], writes=[nk], inc=(dc == 1))
            S.op("dve", lambda: nc.vector.scalar_tensor_tensor(
                out=nst[:, h, :], in0=nst[:, h, :], scalar=gm[:, 40 + h:41 + h], in1=nps[:, 0:2],
                op0=ALU.mult, op1=ALU.add), reads=[nk, gmk, ("n", h)], writes=[("n", h)])

        gm_t = [sb("gm%d" % i, [128, 48], F32) for i in range(2)]
        gm_i = [0]
        ksc_t = [sb("ksc%d" % i, [128, 256], BF16) for i in range(2)]
        ksc_i = [0]
        tp_i = [0]
        wslots = [sb("w%d" % i, [128, KC, WC], BF16) for i in range(NWS)]
        WS = WStream(S, wslots, NWS)

        if phase in (0, 1):
            p1 = ExitStack()
            sb1 = lambda name, shape, dt: p1.enter_context(nc.sbuf_tensor("s1_" + name, shape, dt))
            hT = sb1("hT", [128, KC, 640], BF16)
            vall = sb1("vall", [128, KC * 640], BF16)
            xin = sb1("xin", [128, D], F32)
            hb = sb1("hb", [128, D], BF16)
            pg = sb1("pg", [128, 2, KC], F32)
            dww = sb1("dww", [128, KC, 31], F32)
            cvec = sb1("cvec", [128, 3, KC], F32)
            qkw = sb1("qkw", [128, KC, 4], F32)
            qkb = sb1("qkb", [128, KC], F32)
            gb = sb1("gb", [128, 16], F32)
            hmask = sb1("hmask", [128, 1], F32)
            ucar = sb1("ucar", [128, KC, 30], F32)
            qkcar = sb1("qkcar", [128, KC, 3], F32)
            lnt = sb1("lnt", [128, 3, 640], F32)
            sm = sb1("sm", [128, 64], F32)
            ssy = sb1("ssy", [128, 5, 16], F32)
            uext = [sb1("uext%d" % i, [128, 30 + 640], F32) for i in range(2)]
            cacc = [sb1("cacc%d" % i, [128, 640], F32) for i in range(2)]
            sig = [sb1("sig%d" % i, [128, 512], F32) for i in range(2)]
            sqb = [sb1("sqb%d" % i, [128, 640], BF16) for i in range(2)]
            yst = [sb1("yst%d" % i, [128, WC], F32) for i in range(2)]
            qst = [sb1("qst%d" % i, [128, 640], BF16) for i in range(2)]
            ozst = [sb1("ozst%d" % i, [128, 5, WC], BF16) for i in range(2)]
            gsall = sb1("gsall", [128, 5, 16], F32)

            for t_, s_, k_ in ((pg, pg_d, "pg"), (dww, dww_d, "dww"), (cvec, cvec_d, "cvec"), (qkw, qkw_d, "qkw"), (qkb, qkb_d, "qkb"), (gb, gb_d, "gb"), (hmask, hmask_d, "hmask")):
                S.dma("sp", t_[:], s_, writes=[k_], dkey="c")
            S.op("dve", lambda: nc.vector.memset(ucar[:], 0.0), writes=["ucar"])
            S.op("dve", lambda: nc.vector.memset(qkcar[:], 0.0), writes=["qkcar"])

            gtile = vall[:, 2 * D:4 * D].bitcast(F32)
            vfm = vall[:, :].rearrange("p (c t) -> p c t", c=KC)
            vtm = vall[:, :].rearrange("p (s d) -> p s d", s=5)
            xb2 = vall[:, 0:2 * D].bitcast(F32)

            tl = tiles_p1()[:ntiles]
            for _ in tl:
                ch = []
                for cg in range(16):
                    ch.append((w_in0, cg * WC, WC))
                    ch.append((w_in0, D + cg * WC, WC))
                for cg in range(16):
                    ch.append((w_in0, 2 * D + cg * WC, WC))
                for ob in range(16):
                    ch.append((w_out0, ob * WC, WC))
                for cg in range(16):
                    ch.append((w_in1, cg * WC, WC))
                ch.append((w_in1, 8192, 16))
                for cg in range(16):
                    ch.append((w_in1, 4096 + cg * WC, WC))
                for cg in range(16):
                    ch.append((w_in1, 8208 + cg * WC, WC))
                    ch.append((w_in1, 12304 + cg * WC, WC))
                WS.plan(ch)

            def norm_transpose(src, srckey, layer, s, extra_reads=()):
                S.op("dve", lambda: nc.vector.memset(sm[:, 0:1], 0.0), writes=["sm0"])
                S.op("act", lambda: nc.scalar.activation(out=hb[:], in_=src, func=AF.Square, accum_out=sm[:, 0:1]),
                     reads=[srckey, "sm0"], writes=["hb", "sm0"])
                S.op("dve", lambda: nc.vector.tensor_scalar(out=sm[:, 1:2], in0=sm[:, 0:1], scalar1=1.0 / D, scalar2=EPS, op0=ALU.mult, op1=ALU.add),
                     reads=["sm0"], writes=["sm1"])
                S.op("act", lambda: nc.scalar.activation(out=sm[:, 2:3], in_=sm[:, 1:2], func=AF.Sqrt), reads=["sm1"], writes=["sm2"])
                S.op("dve", lambda: nc.vector.reciprocal(out=sm[:, 2:3], in_=sm[:, 2:3]), reads=["sm2"], writes=["sm2"])
                S.op("act", lambda: nc.scalar.activation(out=hb[:], in_=src, func=AF.Copy, scale=sm[:, 2:3]),
                     reads=[srckey, "sm2"], writes=["hb"])
                for g4 in range(4):
                    bank = tp[tp_i[0] % 2]
                    bkey = ("tp", tp_i[0] % 2)
                    tp_i[0] += 1
                    for j in range(8):
                        kc = g4 * 8 + j
                        S.op("pe", lambda j=j, kc=kc, bank=bank: nc.tensor.transpose(out=bank[:, j * 128:(j + 1) * 128], in_=hb[:, kc * 128:(kc + 1) * 128], identity=ident[:]),
                             reads=["hb", "ident"], writes=[bkey], inc=(j == 7))
                    S.op("dve", lambda g4=g4, bank=bank: nc.vector.tensor_tensor(
                        out=hT[:, g4 * 8:(g4 + 1) * 8, s * 128:(s + 1) * 128],
                        in0=bank[:, :].rearrange("p (j t) -> p j t", j=8),
                        in1=pg[:, layer, g4 * 8:(g4 + 1) * 8].unsqueeze(2).to_broadcast([128, 8, 128]),
                        op=ALU.mult), reads=[bkey, "pg"], writes=[("hT", s)])

            for ti, (sub0, nsub) in enumerate(tl):
                T = nsub * 128
                NB = nblocks(T)
                tok0 = sub0 * 128
                hTkeys = [("hT", s) for s in range(nsub)]
                for s in range(nsub):
                    r0 = tok0 + s * 128
                    S.dma("sp", xin[:], x_d[r0:r0 + 128, :], writes=["xin"], dkey="xin")
                    norm_transpose(xin[:], "xin", 0, s)
                for cg in range(16):
                    wa, wak = WS.next()
                    wg, wgk = WS.next()
                    for c2 in range(2):
                        cb = cg * 2 + c2
                        ue = uext[cb % 2]
                        uk = ("uext", cb % 2)
                        S.op("act", lambda ue=ue, cb=cb: nc.scalar.copy(out=ue[:, 0:30], in_=ucar[:, cb, :]), reads=["ucar"], writes=[uk])
                        for (n0, nl) in NB:
                            pa, pak = next_acc()
                            pgk_ = next_acc()
                            pgs, pgk = pgk_
                            for kc in range(KC):
                                S.op("pe", lambda kc=kc, pa=pa: nc.tensor.matmul(pa[:, 0:nl], lhsT=wa[:, kc, c2 * 128:(c2 + 1) * 128], rhs=hT[:, kc, n0:n0 + nl], start=(kc == 0), stop=(kc == KC - 1)),
                                     reads=[wak] + hTkeys, writes=[pak], inc=(kc == KC - 1))
                            for kc in range(KC):
                                S.op("pe", lambda kc=kc, pgs=pgs: nc.tensor.matmul(pgs[:, 0:nl], lhsT=wg[:, kc, c2 * 128:(c2 + 1) * 128], rhs=hT[:, kc, n0:n0 + nl], start=(kc == 0), stop=(kc == KC - 1)),
                                     reads=[wgk] + hTkeys, writes=[pgk], inc=(kc == KC - 1))
                            sg = sig[acc_i[0] % 2]
                            sgk = ("sig", acc_i[0] % 2)
                            S.op("act", lambda sg=sg, pgs=pgs: nc.scalar.activation(out=sg[:, 0:nl], in_=pgs[:, 0:nl], func=AF.Sigmoid), reads=[pgk], writes=[sgk])
                            S.op("dve", lambda sg=sg, pa=pa, ue=ue: nc.vector.tensor_tensor(out=ue[:, 30 + n0:30 + n0 + nl], in0=pa[:, 0:nl], in1=sg[:, 0:nl], op=ALU.mult),
                                 reads=[pak, sgk], writes=[uk])
                        S.op("act", lambda ue=ue, cb=cb: nc.scalar.copy(out=ucar[:, cb, :], in_=ue[:, T:T + 30]), reads=[uk], writes=["ucar"])
                        ca = cacc[cb % 2]
                        cak = ("cacc", cb % 2)
                        S.op("dve", lambda ue=ue, ca=ca, cb=cb: nc.vector.tensor_scalar(out=ca[:, 0:T], in0=ue[:, 0:T], scalar1=dww[:, cb, 0:1], scalar2=cvec[:, 0, cb:cb + 1], op0=ALU.mult, op1=ALU.add),
                             reads=[uk, "dww", "cvec"], writes=[cak])
                        for j in range(1, 31):
                            last = j == 30
                            outap = vfm[:, cb, 0:T] if last else ca[:, 0:T]
                            S.op("dve", lambda ue=ue, ca=ca, cb=cb, j=j, outap=outap: nc.vector.scalar_tensor_tensor(
                                out=outap, in0=ue[:, j:j + T], scalar=dww[:, cb, j:j + 1], in1=ca[:, 0:T], op0=ALU.mult, op1=ALU.add),
                                reads=[uk, cak], writes=([("v", cb)] if last else [cak]))
                vkeys = [("v", cb) for cb in range(KC)]
                for (n0, nl) in NB:
                    p1_, p1k = next_acc()
                    for cb in range(KC):
                        S.op("pe", lambda cb=cb, p1_=p1_: nc.tensor.matmul(p1_[:, 0:nl], lhsT=onesb[:], rhs=vfm[:, cb, n0:n0 + nl], start=(cb == 0), stop=(cb == KC - 1)),
                             reads=["onesb", ("v", cb)], writes=[p1k], inc=(cb == KC - 1))
                    p2_, p2k = next_acc()
                    for cb in range(KC):
                        sq = sqb[cb % 2]
                        sqk = ("sqb", cb % 2)
                        S.op("act", lambda cb=cb, sq=sq: nc.scalar.activation(out=sq[:, 0:nl], in_=vfm[:, cb, n0:n0 + nl], func=AF.Square), reads=[("v", cb)], writes=[sqk])
                        S.op("pe", lambda cb=cb, sq=sq, p2_=p2_: nc.tensor.matmul(p2_[:, 0:nl], lhsT=onesb[:], rhs=sq[:, 0:nl], start=(cb == 0), stop=(cb == KC - 1)),
                             reads=["onesb", sqk], writes=[p2k], inc=True)
                    S.op("act", lambda p1_=p1_: nc.scalar.activation(out=lnt[:, 0, n0:n0 + nl], in_=p1_[:, 0:nl], func=AF.Copy, scale=1.0 / D), reads=[p1k], writes=["lnt0"])
                    S.op("dve", lambda: nc.vector.tensor_tensor(out=lnt[:, 2, n0:n0 + nl], in0=lnt[:, 0, n0:n0 + nl], in1=lnt[:, 0, n0:n0 + nl], op=ALU.mult), reads=["lnt0"], writes=["lnt2"])
                    S.op("dve", lambda p2_=p2_: nc.vector.scalar_tensor_tensor(out=lnt[:, 1, n0:n0 + nl], in0=p2_[:, 0:nl], scalar=1.0 / D, in1=lnt[:, 2, n0:n0 + nl], op0=ALU.mult, op1=ALU.subtract),
                         reads=[p2k, "lnt2"], writes=["lnt1"])
                    S.op("dve", lambda: nc.vector.tensor_scalar(out=lnt[:, 1, n0:n0 + nl], in0=lnt[:, 1, n0:n0 + nl], scalar1=LN_EPS, scalar2=None, op0=ALU.add), reads=["lnt1"], writes=["lnt1"])
                    S.op("act", lambda: nc.scalar.activation(out=lnt[:, 1, n0:n0 + nl], in_=lnt[:, 1, n0:n0 + nl], func=AF.Sqrt), reads=["lnt1"], writes=["lnt1"])
                    S.op("dve", lambda: nc.vector.reciprocal(out=lnt[:, 1, n0:n0 + nl], in_=lnt[:, 1, n0:n0 + nl]), reads=["lnt1"], writes=["lnt1"])
                    S.op("dve", lambda: nc.vector.tensor_tensor(out=lnt[:, 0, n0:n0 + nl], in0=lnt[:, 0, n0:n0 + nl], in1=lnt[:, 1, n0:n0 + nl], op=ALU.mult), reads=["lnt0", "lnt1"], writes=["lnt0"])
                for cg in range(16):
                    wz, wzk = WS.next()
                    for c2 in range(2):
                        cb = cg * 2 + c2
                        tf = cacc[cb % 2]
                        tfk = ("cacc", cb % 2)
                        S.op("dve", lambda cb=cb, tf=tf: nc.vector.tensor_tensor(out=tf[:, 0:T], in0=vfm[:, cb, 0:T], in1=lnt[:, 1, 0:T], op=ALU.mult), reads=[("v", cb), "lnt1"], writes=[tfk])
                        S.op("dve", lambda cb=cb, tf=tf: nc.vector.tensor_tensor(out=tf[:, 0:T], in0=tf[:, 0:T], in1=lnt[:, 0, 0:T], op=ALU.subtract), reads=[tfk, "lnt0"], writes=[tfk])
                        S.op("act", lambda cb=cb, tf=tf: nc.scalar.activation(out=vfm[:, cb, 0:T], in_=tf[:, 0:T], func=AF.Silu, scale=cvec[:, 1, cb:cb + 1], bias=cvec[:, 2, cb:cb + 1]),
                             reads=[tfk, "cvec"], writes=[("v", cb)])
                        for (n0, nl) in NB:
                            pz, pzk = next_acc()
                            for kc in range(KC):
                                S.op("pe", lambda kc=kc, pz=pz: nc.tensor.matmul(pz[:, 0:nl], lhsT=wz[:, kc, c2 * 128:(c2 + 1) * 128], rhs=hT[:, kc, n0:n0 + nl], start=(kc == 0), stop=(kc == KC - 1)),
                                     reads=[wzk] + hTkeys, writes=[pzk], inc=(kc == KC - 1))
                            sq = sqb[acc_i[0] % 2]
                            sqk = ("sqb", acc_i[0] % 2)
                            S.op("act", lambda sq=sq, pz=pz: nc.scalar.activation(out=sq[:, 0:nl], in_=pz[:, 0:nl], func=AF.Silu), reads=[pzk], writes=[sqk])
                            S.op("dve", lambda sq=sq, cb=cb: nc.vector.tensor_tensor(out=vfm[:, cb, n0:n0 + nl], in0=vfm[:, cb, n0:n0 + nl], in1=sq[:, 0:nl], op=ALU.mult),
                                 reads=[sqk, ("v", cb)], writes=[("v", cb)])
                S.op("dve", lambda: nc.vector.memset(ssy[:], 0.0), writes=["ssy"])
                for ob in range(16):
                    wo, wok = WS.next()
                    for s in range(nsub):
                        py, pyk = next_acc()
                        for kc in range(KC):
                            S.op("pe", lambda kc=kc, py=py, s=s: nc.tensor.matmul(py[:, 0:WC], lhsT=vfm[:, kc, s * 128:(s + 1) * 128], rhs=wo[:, kc, :], start=(kc == 0), stop=(kc == KC - 1)),
                                 reads=[wok] + vkeys, writes=[pyk], inc=(kc == KC - 1))
                        yt = yst[acc_i[0] % 2]
                        ytk = ("yst", acc_i[0] % 2)
                        S.op("act", lambda yt=yt, py=py: nc.scalar.copy(out=yt[:], in_=py[:, 0:WC]), reads=[pyk], writes=[ytk])
                        S.op("act", lambda py=py, s=s, ob=ob: nc.scalar.activation(out=sqb[0][:, 0:WC], in_=py[:, 0:WC], func=AF.Square, accum_out=ssy[:, s, ob:ob + 1]),
                             reads=[pyk, "ssy"], writes=["ssy", ("sqb", 0)])
                        r0 = tok0 + s * 128
                        S.dma("sp", ys[r0:r0 + 128, ob * WC:(ob + 1) * WC], yt[:], reads=[ytk], writes=[("ys", s)], dkey="yst%d" % (acc_i[0] % 2))
                S.dma("sp", gtile, postg_d[0], writes=vkeys + ["gtile"], reads=[], dkey="gt")
                for s in range(nsub):
                    r0 = tok0 + s * 128
                    S.dma("sp", xin[:], ys[r0:r0 + 128, :], reads=[("ys", s)], writes=["xin"], dkey="xin")
                    S.dma("sp", xb2, x_d[r0:r0 + 128, :], writes=vkeys + ["xb2"], dkey="xb2")
                    S.op("dve", lambda s=s: nc.vector.tensor_reduce(out=sm[:, 8:9], in_=ssy[:, s, :], axis=mybir.AxisListType.X, op=ALU.add), reads=["ssy"], writes=["sm8"])
                    S.op("dve", lambda: nc.vector.tensor_scalar(out=sm[:, 9:10], in0=sm[:, 8:9], scalar1=1.0 / D, scalar2=EPS, op0=ALU.mult, op1=ALU.add), reads=["sm8"], writes=["sm9"])
                    S.op("act", lambda: nc.scalar.activation(out=sm[:, 10:11], in_=sm[:, 9:10], func=AF.Sqrt), reads=["sm9"], writes=["sm10"])
                    S.op("dve", lambda: nc.vector.reciprocal(out=sm[:, 10:11], in_=sm[:, 10:11]), reads=["sm10"], writes=["sm10"])
                    S.op("dve", lambda: nc.vector.scalar_tensor_tensor(out=xin[:], in0=xin[:], scalar=sm[:, 10:11], in1=gtile, op0=ALU.mult, op1=ALU.mult),
                         reads=["xin", "sm10", "gtile"], writes=["xin"])
                    S.op("dve", lambda: nc.vector.tensor_tensor(out=xin[:], in0=xin[:], in1=xb2, op=ALU.add), reads=["xin", "xb2"], writes=["xin"])
                    S.dma("sp", x1s[r0:r0 + 128, :], xin[:], reads=["xin"], writes=[("x1s", sub0 + s)], dkey="x1st")
                    norm_transpose(xin[:], "xin", 1, s)
                for cg in range(16):
                    wq, wqk = WS.next()
                    for c2 in range(2):
                        cb = cg * 2 + c2
                        ue = uext[cb % 2]
                        uk = ("uext", cb % 2)
                        S.op("act", lambda ue=ue, cb=cb: nc.scalar.copy(out=ue[:, 0:3], in_=qkcar[:, cb, :]), reads=["qkcar"], writes=[uk])
                        for (n0, nl) in NB:
                            pq, pqk = next_acc()
                            for kc in range(KC):
                                S.op("pe", lambda kc=kc, pq=pq: nc.tensor.matmul(pq[:, 0:nl], lhsT=wq[:, kc, c2 * 128:(c2 + 1) * 128], rhs=hT[:, kc, n0:n0 + nl], start=(kc == 0), stop=(kc == KC - 1)),
                                     reads=[wqk] + hTkeys, writes=[pqk], inc=(kc == KC - 1))
                            S.op("act", lambda ue=ue, pq=pq: nc.scalar.copy(out=ue[:, 3 + n0:3 + n0 + nl], in_=pq[:, 0:nl]), reads=[pqk], writes=[uk])
                        if ti == 0:
                            S.op("dve", lambda ue=ue: nc.vector.tensor_scalar(out=ue[:, 3:3 + 128], in0=ue[:, 3:3 + 128], scalar1=hmask[:, 0:1], scalar2=None, op0=ALU.mult),
                                 reads=[uk, "hmask"], writes=[uk])
                        S.op("act", lambda ue=ue, cb=cb: nc.scalar.copy(out=qkcar[:, cb, :], in_=ue[:, T:T + 3]), reads=[uk], writes=["qkcar"])
                        ca = cacc[cb % 2]
                        cak = ("cacc", cb % 2)
                        S.op("dve", lambda ue=ue, ca=ca, cb=cb: nc.vector.tensor_scalar(out=ca[:, 0:T], in0=ue[:, 0:T], scalar1=qkw[:, cb, 0:1], scalar2=qkb[:, cb:cb + 1], op0=ALU.mult, op1=ALU.add),
                             reads=[uk, "qkw", "qkb"], writes=[cak])
                        for j in range(1, 4):
                            S.op("dve", lambda ue=ue, ca=ca, cb=cb, j=j: nc.vector.scalar_tensor_tensor(
                                out=ca[:, 0:T], in0=ue[:, j:j + T], scalar=qkw[:, cb, j:j + 1], in1=ca[:, 0:T], op0=ALU.mult, op1=ALU.add),
                                reads=[uk, cak], writes=[cak])
                        if cb < 16:
                            qt = qst[cb % 2]
                            qtk = ("qst", cb % 2)
                            S.op("act", lambda qt=qt, ca=ca: nc.scalar.activation(out=qt[:, 0:T], in_=ca[:, 0:T], func=AF.Silu), reads=[cak], writes=[qtk])
                            S.op("dve", lambda qt=qt: nc.vector.tensor_scalar(out=qt[:, 0:T], in0=qt[:, 0:T], scalar1=0.0625, scalar2=None, op0=ALU.mult), reads=[qtk], writes=[qtk])
                            S.dma("sp", qs[cb, :, tok0:tok0 + T], qt[:, 0:T], reads=[qtk], writes=[("qs", ti)], dkey="qst%d" % (cb % 2))
                        else:
                            qt = qst[cb % 2]
                            qtk = ("qst", cb % 2)
                            S.op("act", lambda qt=qt, ca=ca: nc.scalar.activation(out=qt[:, 0:T], in_=ca[:, 0:T], func=AF.Silu), reads=[cak], writes=[qtk])
                            S.dma("sp", ks[cb - 16, :, tok0:tok0 + T], qt[:, 0:T], reads=[qtk], writes=[("ks", ti)], dkey="qst%d" % (cb % 2))
                wgt, wgtk = WS.next()
                gsbs = []
                for s in range(nsub):
                    pgt, pgtk = next_acc()
                    for kc in range(KC):
                        S.op("pe", lambda kc=kc, pgt=pgt, s=s: nc.tensor.matmul(pgt[:, 0:16], lhsT=hT[:, kc, s * 128:(s + 1) * 128], rhs=wgt[:, kc, 0:16], start=(kc == 0), stop=(kc == KC - 1)),
                             reads=[wgtk] + hTkeys, writes=[pgtk], inc=(kc == KC - 1))
                    g_ = gsall[:, s, :]
                    S.op("dve", lambda pgt=pgt, g_=g_: nc.vector.tensor_tensor(out=g_, in0=pgt[:, 0:16], in1=gb[:], op=ALU.add), reads=[pgtk, "gb"], writes=[("gs", s)])
                    S.op("act", lambda g_=g_: nc.scalar.activation(out=g_[:, 8:16], in_=g_[:, 8:16], func=AF.Sigmoid), reads=[("gs", s)], writes=[("gs", s)])
                    S.op("act", lambda g_=g_: nc.scalar.activation(out=g_[:, 8:16], in_=g_[:, 8:16], func=AF.Ln), reads=[("gs", s)], writes=[("gs", s)])
                    r0 = tok0 + s * 128
                    S.dma("sp", gs[r0:r0 + 128, :], g_, reads=[("gs", s)], writes=[("gsd", sub0 + s)], dkey="gst")
                for cg in range(16):
                    wv, wvk = WS.next()
                    for s in range(nsub):
                        pv, pvk = next_acc()
                        for kc in range(KC):
                            S.op("pe", lambda kc=kc, pv=pv, s=s: nc.tensor.matmul(pv[:, 0:WC], lhsT=hT[:, kc, s * 128:(s + 1) * 128], rhs=wv[:, kc, :], start=(kc == 0), stop=(kc == KC - 1)),
                                 reads=[wvk] + hTkeys, writes=[pvk], inc=(kc == KC - 1))
                        S.op("act", lambda pv=pv, s=s, cg=cg: nc.scalar.copy(out=vtm[:, s, cg * WC:(cg + 1) * WC], in_=pv[:, 0:WC]), reads=[pvk], writes=[("vtm", s), "xb2", "gtile"] + vkeys)
                for s in range(nsub):
                    r0 = tok0 + s * 128
                    S.dma("sp", vs[r0:r0 + 128, :], vtm[:, s, :], reads=[("vtm", s)] + vkeys, writes=[("vs", sub0 + s)], dkey="vst")
                for cg in range(16):
                    wo_, wok_ = WS.next()
                    wz_, wzk_ = WS.next()
                    ot = ozst[cg % 2]
                    otk = ("ozst", cg % 2)
                    for s in range(nsub):
                        po, pok = next_acc()
                        pz, pzk = next_acc()
                        for kc in range(KC):
                            S.op("pe", lambda kc=kc, po=po, s=s: nc.tensor.matmul(po[:, 0:WC], lhsT=hT[:, kc, s * 128:(s + 1) * 128], rhs=wo_[:, kc, :], start=(kc == 0), stop=(kc == KC - 1)),
                                 reads=[wok_] + hTkeys, writes=[pok], inc=(kc == KC - 1))
                        for kc in range(KC):
                            S.op("pe", lambda kc=kc, pz=pz, s=s: nc.tensor.matmul(pz[:, 0:WC], lhsT=hT[:, kc, s * 128:(s + 1) * 128], rhs=wz_[:, kc, :], start=(kc == 0), stop=(kc == KC - 1)),
                                 reads=[wzk_] + hTkeys, writes=[pzk], inc=(kc == KC - 1))
                        sg = sig[acc_i[0] % 2]
                        sgk = ("sig", acc_i[0] % 2)
                        S.op("act", lambda sg=sg, po=po: nc.scalar.activation(out=sg[:, 0:WC], in_=po[:, 0:WC], func=AF.Sigmoid), reads=[pok], writes=[sgk])
                        S.op("act", lambda sg=sg, pz=pz: nc.scalar.activation(out=sg[:, WC:2 * WC], in_=pz[:, 0:WC], func=AF.Silu), reads=[pzk], writes=[sgk])
                        S.op("dve", lambda sg=sg, ot=ot, s=s: nc.vector.tensor_tensor(out=ot[:, s, :], in0=sg[:, 0:WC], in1=sg[:, WC:2 * WC], op=ALU.mult),
                             reads=[sgk], writes=[otk])
                    c0 = cg * WC
                    S.dma("sp", ozs[tok0:tok0 + T, c0:c0 + WC].rearrange("(s p) c -> p s c", p=128), ot[:, 0:nsub, :], reads=[otk], writes=[("ozs", ti)], dkey="ozst%d" % (cg % 2))
            p1.close()
            p15 = ExitStack()
            sb15 = lambda name, shape, dt: p15.enter_context(nc.sbuf_tensor("s15_" + name, shape, dt))
            Cst = sb15("Cst", [128, NH, 2, 512], F32)
            nst = sb15("nst", [128, NH, 2], F32)
            gacc = sb15("gacc", [128, NH], F32)
            kch = [sb15("kch%d" % i, [128, 16, 128], BF16) for i in range(2)]
            vch = [sb15("vch%d" % i, [128, D], BF16) for i in range(2)]
            gch = [sb15("gch%d" % i, [128, 16], F32) for i in range(2)]
            S.op("dve", lambda: nc.vector.memset(Cst[:], 0.0), writes=[("C", h) for h in range(NH)])
            S.op("dve", lambda: nc.vector.memset(nst[:], 0.0), writes=[("n", h) for h in range(NH)])
            S.op("dve", lambda: nc.vector.memset(gacc[:], 0.0), writes=["gacc"])
            nchunks = ntok1 // 128
            for c in range(1, nchunks):
                b = c % 2
                dep_t = [("ks", t) for t in range(len(tl))]
                S.dma("sp", kch[b][:], ks[:, :, c * 128:(c + 1) * 128].rearrange("b p t -> p b t"), reads=dep_t, writes=[("kch", b)], dkey="kch%d" % b)
                S.dma("sp", vch[b][:], vs[c * 128:(c + 1) * 128, :], reads=[("vs", c)], writes=[("vch", b)], dkey="vch%d" % b)
                S.dma("sp", gch[b][:], gs[c * 128:(c + 1) * 128, :], reads=[("gsd", c)], writes=[("gch", b)], dkey="gch%d" % b)
                gm, gmk = gate_math(gch[b][:], ("gch", b))
                for h in range(NH):
                    state_update(h, lambda dc, h=h, b=b: kch[b][:, 2 * h + dc, :], ("kch", b), vch[b][:, h * 512:(h + 1) * 512], ("vch", b), gm, gmk, Cst, nst)
                S.op("dve", lambda gm=gm: nc.vector.tensor_tensor(out=gacc[:], in0=gacc[:], in1=gm[:, 8:16], op=ALU.add), reads=[gmk, "gacc"], writes=["gacc"])
            S.dma("sp", cloc[:, :], Cst[:].rearrange("p h d v -> p (h d v)"), reads=[("C", h) for h in range(NH)], writes=["cloc"], dkey="fin")
            S.dma("sp", nloc[:, :], nst[:].rearrange("p h d -> p (h d)"), reads=[("n", h) for h in range(NH)], writes=["nloc"], dkey="fin")
            S.dma("sp", gtot[:, :], gacc[:], reads=["gacc"], writes=["gtot"], dkey="fin")
            S.wait_all("sp")
            p15.close()


        if phase in (0, 2):
            p2 = ExitStack()
            sb2 = lambda name, shape, dt: p2.enter_context(nc.sbuf_tensor("s2_" + name, shape, dt))
            Cst = sb2("Cst2", [128, NH, 2, 512], F32)
            nst = sb2("nst2", [128, NH, 2], F32)
            Cbf = sb2("Cbf", [128, NH, 2, 512], BF16)
            nbf = sb2("nbf", [128, NH, 2], BF16)
            qk = sb2("qk", [128, 2, 16, 512], BF16)
            vch = sb2("vch2", [128, D], BF16)
            gch = [sb2("gch2%d" % i, [128, 16], F32) for i in range(2)]
            gtm = sb2("gtm", [128, D], BF16)
            gT = sb2("gT", [128, KC, 512], BF16)
            gtile = sb2("gtile2", [128, D], F32)
            smk = sb2("smk", [128, 8, 16], F32)
            Sm = [sb2("Sm%d" % i, [128, 128], BF16) for i in range(2)]
            t1 = [sb2("t1%d" % i, [128, 512], F32) for i in range(2)]
            junk = sb2("junk", [128, 512], BF16)
            yst2 = [sb2("yst2%d" % i, [128, WC], F32) for i in range(2)]
            ssy2 = sb2("ssy2", [128, 4, 16], F32)
            cmask = sb2("cmask", [128, 4, 4], F32)
            cvalid = sb2("cvalid", [128, 4], F32)
            gta = sb2("gta", [128, 4, NH], F32)
            cf = sb2("cf", [128, 4, NH], F32)
            nal = sb2("nal", [128, 4, 16], F32)
            ybuf = qk[:, 0, :, :].rearrange("p b t -> p (b t)")[:, 0:2 * D].bitcast(F32)
            xbuf = qk[:, 1, :, :].rearrange("p b t -> p (b t)")[:, 0:2 * D].bitcast(F32)
            stage = qk[:, :, :, :].rearrange("p a b t -> p (a b t)")[:, 0:4 * D].bitcast(F32)
            qkkeys = ["qT", "kT", "ybuf", "xbuf"]

            S.dma("sp", cmask[:], cmask_d, writes=["cmask"], dkey="c")
            S.dma("sp", cvalid[:], cvalid_d, writes=["cvalid"], dkey="c")
            S.dma("sp", gta[:], gtall, writes=["gta"], dkey="c")
            S.dma("sp", nal[:], nall.rearrange("i p c -> p i c"), writes=["nal"], dkey="c")
            S.op("dve", lambda: nc.vector.memset(Cst[:], 0.0), writes=[("C", h) for h in range(NH)])
            S.op("dve", lambda: nc.vector.memset(nst[:], 0.0), writes=[("n", h) for h in range(NH)])
            for i in range(3):
                S.op("dve", lambda i=i: nc.vector.tensor_scalar(out=cf[:, i, :], in0=gta[:, 0, :], scalar1=cmask[:, i, 0:1], scalar2=None, op0=ALU.mult), reads=["gta", "cmask"], writes=["cf"])
                for m in range(1, 4):
                    S.op("dve", lambda i=i, m=m: nc.vector.scalar_tensor_tensor(out=cf[:, i, :], in0=gta[:, m, :], scalar=cmask[:, i, m:m + 1], in1=cf[:, i, :], op0=ALU.mult, op1=ALU.add),
                         reads=["gta", "cmask", "cf"], writes=["cf"])
                S.op("act", lambda i=i: nc.scalar.activation(out=cf[:, i, :], in_=cf[:, i, :], func=AF.Exp), reads=["cf"], writes=["cf"])
                S.op("dve", lambda i=i: nc.vector.tensor_scalar(out=cf[:, i, :], in0=cf[:, i, :], scalar1=cvalid[:, i:i + 1], scalar2=None, op0=ALU.mult), reads=["cf", "cvalid"], writes=["cf"])
                S.dma("sp", stage, call[i], writes=qkkeys, dkey="stage")
                for h in range(NH):
                    S.op("dve", lambda i=i, h=h: nc.vector.scalar_tensor_tensor(
                        out=Cst[:, h, :, :].rearrange("p d v -> p (d v)"), in0=stage[:, h * 1024:(h + 1) * 1024], scalar=cf[:, i, h:h + 1],
                        in1=Cst[:, h, :, :].rearrange("p d v -> p (d v)"), op0=ALU.mult, op1=ALU.add),
                        reads=qkkeys + ["cf", ("C", h)], writes=[("C", h)])
                    S.op("dve", lambda i=i, h=h: nc.vector.scalar_tensor_tensor(
                        out=nst[:, h, :], in0=nal[:, i, 2 * h:2 * h + 2], scalar=cf[:, i, h:h + 1], in1=nst[:, h, :], op0=ALU.mult, op1=ALU.add),
                        reads=["nal", "cf", ("n", h)], writes=[("n", h)])
            for h in range(NH):
                S.op("act", lambda h=h: nc.scalar.copy(out=Cbf[:, h, :, :], in_=Cst[:, h, :, :]), reads=[("C", h)], writes=[("Cbf", h)])
                S.op("act", lambda h=h: nc.scalar.copy(out=nbf[:, h, :], in_=nst[:, h, :]), reads=[("n", h)], writes=[("nbf", h)])

            for _ in range(4):
                WS.plan([(w_out1, ob * WC, WC) for ob in range(16)])

            for ti in range(4):
                tk0 = HALO + ti * 512
                o0 = ti * 512
                S.dma("sp", qk[:, 0, :, :], qs[:, :, tk0:tk0 + 512].rearrange("b p t -> p b t"), reads=[("qs", t) for t in range(4)], writes=["qT", "ybuf"], dkey="qT")
                S.dma("sp", qk[:, 1, :, :], ks[:, :, tk0:tk0 + 512].rearrange("b p t -> p b t"), reads=[("ks", t) for t in range(4)], writes=["kT", "xbuf"], dkey="kT")
                S.dma("sp", gtile[:], hng_d, writes=["gtile"], dkey="gt2")
                for c in range(4):
                    r0 = tk0 + c * 128
                    gb_ = gch[c % 2]
                    gk = ("gch", c % 2)
                    S.dma("sp", vch[:], vs[r0:r0 + 128, :], reads=[("vs", r0 // 128)], writes=["vch"], dkey="vch2")
                    S.dma("sp", gtm[:], ozs[r0:r0 + 128, :], reads=[("ozs", t) for t in range(4)], writes=["gtm"], dkey="ozch")
                    S.dma("sp", gb_[:], gs[r0:r0 + 128, :], reads=[("gsd", r0 // 128)], writes=[gk], dkey="gch2%d" % (c % 2))
                    gm, gmk = gate_math(gb_[:], gk)
                    for h in range(NH):
                        qc = lambda dc, h=h, c=c: qk[:, 0, 2 * h + dc, c * 128:(c + 1) * 128]
                        kc_ = lambda dc, h=h, c=c: qk[:, 1, 2 * h + dc, c * 128:(c + 1) * 128]
                        sp_, spk = next_acc()
                        for dc in range(2):
                            S.op("pe", lambda dc=dc, sp_=sp_: nc.tensor.matmul(sp_[:, 0:128], lhsT=kc_(dc), rhs=qc(dc), start=(dc == 0), stop=(dc == 1)),
                                 reads=["qT", "kT"], writes=[spk], inc=(dc == 1))
                        sm_ = Sm[h % 2]
                        smk_ = ("Sm", h % 2)
                        S.op("dve", lambda sp_=sp_, sm_=sm_, h=h, gm=gm: nc.vector.scalar_tensor_tensor(out=sm_[:], in0=sp_[:, 0:128], scalar=gm[:, 24 + h:25 + h], in1=tri[:], op0=ALU.mult, op1=ALU.mult),
                             reads=[spk, gmk, "tri"], writes=[smk_])
                        np_, npk = next_acc()
                        S.op("pe", lambda np_=np_, sm_=sm_, h=h: nc.tensor.matmul(np_[:, 0:512], lhsT=sm_[:], rhs=vch[:, h * 512:(h + 1) * 512], start=True, stop=False),
                             reads=[smk_, "vch"], writes=[npk], inc=False)
                        for dc in range(2):
                            S.op("pe", lambda dc=dc, np_=np_, h=h: nc.tensor.matmul(np_[:, 0:512], lhsT=qc(dc), rhs=Cbf[:, h, dc, :], start=False, stop=(dc == 1)),
                                 reads=["qT", ("Cbf", h)], writes=[npk], inc=(dc == 1))
                        dp_, dpk = next_acc()
                        S.op("pe", lambda dp_=dp_, sm_=sm_: nc.tensor.matmul(dp_[:, 0:1], lhsT=sm_[:], rhs=onesb[:, 0:1], start=True, stop=False),
                             reads=[smk_, "onesb"], writes=[dpk], inc=False)
                        for dc in range(2):
                            S.op("pe", lambda dc=dc, dp_=dp_, h=h: nc.tensor.matmul(dp_[:, 0:1], lhsT=qc(dc), rhs=nbf[:, h, dc:dc + 1], start=False, stop=(dc == 1)),
                                 reads=["qT", ("nbf", h)], writes=[dpk], inc=(dc == 1))
                        k_ = ("smk", h)
                        S.op("dve", lambda dp_=dp_, h=h, gm=gm: nc.vector.tensor_tensor(out=smk[:, h, 0:1], in0=dp_[:, 0:1], in1=gm[:, 32 + h:33 + h], op=ALU.mult), reads=[dpk, gmk], writes=[k_])
                        S.op("act", lambda h=h: nc.scalar.activation(out=smk[:, h, 1:2], in_=smk[:, h, 0:1], func=AF.Abs), reads=[k_], writes=[k_])
                        S.op("dve", lambda h=h: nc.vector.tensor_scalar(out=smk[:, h, 1:2], in0=smk[:, h, 1:2], scalar1=1.0, scalar2=None, op0=ALU.max), reads=[k_], writes=[k_])
                        S.op("dve", lambda h=h: nc.vector.reciprocal(out=smk[:, h, 2:3], in_=smk[:, h, 1:2]), reads=[k_], writes=[k_])
                        S.op("dve", lambda h=h, gm=gm: nc.vector.tensor_tensor(out=smk[:, h, 2:3], in0=gm[:, 32 + h:33 + h], in1=smk[:, h, 2:3], op=ALU.mult), reads=[k_, gmk], writes=[k_])
                        S.op("dve", lambda h=h: nc.vector.memset(smk[:, h, 3:4], 0.0), writes=[k_])
                        S.op("act", lambda np_=np_, h=h: nc.scalar.activation(out=junk[:], in_=np_[:, 0:512], func=AF.Square, accum_out=smk[:, h, 3:4]), reads=[npk, k_], writes=[k_, "junk"])
                        S.op("dve", lambda h=h: nc.vector.tensor_tensor(out=smk[:, h, 4:5], in0=smk[:, h, 2:3], in1=smk[:, h, 2:3], op=ALU.mult), reads=[k_], writes=[k_])
                        S.op("dve", lambda h=h: nc.vector.scalar_tensor_tensor(out=smk[:, h, 4:5], in0=smk[:, h, 3:4], scalar=1.0 / 512, in1=smk[:, h, 4:5], op0=ALU.mult, op1=ALU.mult), reads=[k_], writes=[k_])
                        S.op("dve", lambda h=h: nc.vector.tensor_scalar(out=smk[:, h, 5:6], in0=smk[:, h, 4:5], scalar1=EPS, scalar2=None, op0=ALU.add), reads=[k_], writes=[k_])
                        S.op("act", lambda h=h: nc.scalar.activation(out=smk[:, h, 5:6], in_=smk[:, h, 5:6], func=AF.Sqrt), reads=[k_], writes=[k_])
                        S.op("dve", lambda h=h: nc.vector.reciprocal(out=smk[:, h, 5:6], in_=smk[:, h, 5:6]), reads=[k_], writes=[k_])
                        S.op("dve", lambda h=h: nc.vector.tensor_tensor(out=smk[:, h, 6:7], in0=smk[:, h, 5:6], in1=smk[:, h, 2:3], op=ALU.mult), reads=[k_], writes=[k_])
                        tt = t1[h % 2]
                        ttk = ("t1", h % 2)
                        S.op("dve", lambda np_=np_, h=h, tt=tt: nc.vector.scalar_tensor_tensor(out=tt[:], in0=np_[:, 0:512], scalar=smk[:, h, 6:7], in1=gtile[:, h * 512:(h + 1) * 512], op0=ALU.mult, op1=ALU.mult),
                             reads=[npk, k_, "gtile"], writes=[ttk])
                        S.op("dve", lambda h=h, tt=tt: nc.vector.tensor_tensor(out=gtm[:, h * 512:(h + 1) * 512], in0=tt[:], in1=gtm[:, h * 512:(h + 1) * 512], op=ALU.mult),
                             reads=[ttk, "gtm"], writes=["gtm"])
                        state_update(h, kc_, "kT", vch[:, h * 512:(h + 1) * 512], "vch", gm, gmk, Cst, nst)
                        S.op("act", lambda h=h: nc.scalar.copy(out=Cbf[:, h, :, :], in_=Cst[:, h, :, :]), reads=[("C", h)], writes=[("Cbf", h)])
                        S.op("act", lambda h=h: nc.scalar.copy(out=nbf[:, h, :], in_=nst[:, h, :]), reads=[("n", h)], writes=[("nbf", h)])
                    for g4 in range(4):
                        bank = tp[tp_i[0] % 2]
                        bkey = ("tp", tp_i[0] % 2)
                        tp_i[0] += 1
                        for j in range(8):
                            kc = g4 * 8 + j
                            S.op("pe", lambda j=j, kc=kc, bank=bank: nc.tensor.transpose(out=bank[:, j * 128:(j + 1) * 128], in_=gtm[:, kc * 128:(kc + 1) * 128], identity=ident[:]),
                                 reads=["gtm", "ident"], writes=[bkey], inc=(j == 7))
                        S.op("act", lambda g4=g4, bank=bank, c=c: nc.scalar.copy(out=gT[:, g4 * 8:(g4 + 1) * 8, c * 128:(c + 1) * 128], in_=bank[:, :].rearrange("p (j t) -> p j t", j=8)),
                             reads=[bkey], writes=[("gT", c)])
                gTkeys = [("gT", c) for c in range(4)]
                S.op("dve", lambda: nc.vector.memset(ssy2[:], 0.0), writes=["ssy2"])
                for ob in range(16):
                    wo, wok = WS.next()
                    for s in range(4):
                        py, pyk = next_acc()
                        for kc in range(KC):
                            S.op("pe", lambda kc=kc, py=py, s=s: nc.tensor.matmul(py[:, 0:WC], lhsT=gT[:, kc, s * 128:(s + 1) * 128], rhs=wo[:, kc, :], start=(kc == 0), stop=(kc == KC - 1)),
                                 reads=[wok] + gTkeys, writes=[pyk], inc=(kc == KC - 1))
                        yt = yst2[acc_i[0] % 2]
                        ytk = ("yst2", acc_i[0] % 2)
                        S.op("act", lambda yt=yt, py=py: nc.scalar.copy(out=yt[:], in_=py[:, 0:WC]), reads=[pyk], writes=[ytk])
                        S.op("act", lambda py=py, s=s, ob=ob: nc.scalar.activation(out=junk[:, 0:WC], in_=py[:, 0:WC], func=AF.Square, accum_out=ssy2[:, s, ob:ob + 1]),
                             reads=[pyk, "ssy2"], writes=["ssy2", "junk"])
                        S.dma("sp", y2s[o0 + s * 128:o0 + (s + 1) * 128, ob * WC:(ob + 1) * WC], yt[:], reads=[ytk], writes=[("y2s", s)], dkey="yst2%d" % (acc_i[0] % 2))
                S.dma("sp", gtile[:], postg_d[1], writes=["gtile"], dkey="gt2")
                for s in range(4):
                    S.dma("sp", ybuf, y2s[o0 + s * 128:o0 + (s + 1) * 128, :], reads=[("y2s", s)], writes=["ybuf", "qT"], dkey="ybuf")
                    S.dma("sp", xbuf, x1s[tk0 + s * 128:tk0 + (s + 1) * 128, :], reads=[("x1s", (tk0 // 128) + s)], writes=["xbuf", "kT"], dkey="xbuf")
                    S.op("dve", lambda s=s: nc.vector.tensor_reduce(out=smk[:, 0, 8:9], in_=ssy2[:, s, :], axis=mybir.AxisListType.X, op=ALU.add), reads=["ssy2"], writes=["r8"])
                    S.op("dve", lambda: nc.vector.tensor_scalar(out=smk[:, 0, 9:10], in0=smk[:, 0, 8:9], scalar1=1.0 / D, scalar2=EPS, op0=ALU.mult, op1=ALU.add), reads=["r8"], writes=["r9"])
                    S.op("act", lambda: nc.scalar.activation(out=smk[:, 0, 10:11], in_=smk[:, 0, 9:10], func=AF.Sqrt), reads=["r9"], writes=["r10"])
                    S.op("dve", lambda: nc.vector.reciprocal(out=smk[:, 0, 10:11], in_=smk[:, 0, 10:11]), reads=["r10"], writes=["r10"])
                    S.op("dve", lambda: nc.vector.scalar_tensor_tensor(out=ybuf, in0=ybuf, scalar=smk[:, 0, 10:11], in1=gtile[:], op0=ALU.mult, op1=ALU.mult), reads=["ybuf", "r10", "gtile"], writes=["ybuf"])
                    S.op("dve", lambda: nc.vector.tensor_tensor(out=ybuf, in0=ybuf, in1=xbuf, op=ALU.add), reads=["ybuf", "xbuf"], writes=["ybuf"])
                    S.dma("sp", out_d[o0 + s * 128:o0 + (s + 1) * 128, :], ybuf, reads=["ybuf"], writes=["outd"], dkey="outd")
            S.wait_all("sp")
            p2.close()

        S.wait_all("sp")
    return nc


def _consts():
    ident = np.eye(128, dtype=np.float32).astype(ml_dtypes.bfloat16)
    tri = np.triu(np.ones((128, 128), dtype=np.float32))
    return ident, tri


def _pp(v):
    return np.ascontiguousarray(v.reshape(KC, 128).T)


def _bc(v):
    return np.ascontiguousarray(np.broadcast_to(v.reshape(1, -1), (128, v.size))).astype(np.float32)


_NC_CACHE = {}


def _get_nc(phase, ntiles=4):
    key = (phase, ntiles)
    if key not in _NC_CACHE:
        _NC_CACHE[key] = build(phase, ntiles)
    return _NC_CACHE[key]


def kernel(x, pre_norm_g, post_norm_g, conv_w_in, conv_dw_w, conv_dw_b, conv_ln_g, conv_ln_b, conv_w_out,
           mlstm_w_in, mlstm_qk_conv_w, mlstm_qk_conv_b, mlstm_gate_b, mlstm_head_norm_g, mlstm_w_out, _ntiles=4, _debug=None):
    f = lambda a: np.ascontiguousarray(np.asarray(a, dtype=np.float32))
    x = f(x)
    ident, tri = _consts()
    hng = _bc(f(mlstm_head_norm_g).reshape(-1))
    postg = np.ascontiguousarray(np.stack([_bc(f(post_norm_g)[0]), _bc(f(post_norm_g)[1])]))
    pg = np.ascontiguousarray(np.stack([_pp(f(pre_norm_g)[0]), _pp(f(pre_norm_g)[1])], axis=1))
    dww = np.ascontiguousarray(f(conv_dw_w)[0].reshape(31, KC, 128).transpose(2, 1, 0))
    cvec = np.ascontiguousarray(np.stack([_pp(f(conv_dw_b)[0]), _pp(f(conv_ln_g)[0]), _pp(f(conv_ln_b)[0])], axis=1))
    qkw = np.ascontiguousarray(f(mlstm_qk_conv_w)[0].reshape(4, KC, 128).transpose(2, 1, 0))
    qkb = _pp(f(mlstm_qk_conv_b)[0])
    gb = _bc(f(mlstm_gate_b)[0])
    w_in0 = f(conv_w_in)[0]
    w_out0 = f(conv_w_out)[0]
    w_in1 = f(mlstm_w_in)[0]
    w_out1 = f(mlstm_w_out)[0]
    zeros_halo = np.zeros((HALO, D), np.float32)
    in1 = []
    for c in range(8):
        b, j = c // 4, c % 4
        t0 = j * NSEG
        xs = np.concatenate([x[b, t0 - HALO:t0] if j > 0 else zeros_halo, x[b, t0:t0 + NSEG]], axis=0)
        in1.append({"x": np.ascontiguousarray(xs), "w_in0": w_in0, "w_out0": w_out0, "w_in1": w_in1, "pg": pg, "dww": dww,
                    "cvec": cvec, "qkw": qkw, "qkb": qkb, "gb": gb, "hmask": np.full((128, 1), 1.0 if j > 0 else 0.0, np.float32),
                    "ident": ident, "tri": tri, "hng": hng, "postg": postg})
    nc1 = _get_nc(1, _ntiles)
    r1 = run_bass_kernel_spmd(nc1, in1, core_ids=list(range(8))).results
    if _debug == 1:
        return r1
    in2 = []
    for c in range(8):
        b, j = c // 4, c % 4
        grp = [r1[b * 4 + i] for i in range(4)]
        cmask = np.zeros((128, 4, 4), np.float32)
        cvalid = np.zeros((128, 4), np.float32)
        for i in range(4):
            if i < j:
                cvalid[:, i] = 1.0
                for m in range(4):
                    if i < m < j:
                        cmask[:, i, m] = 1.0
        in2.append({"x1s": r1[c]["x1s"], "qs": r1[c]["qs"], "ks": r1[c]["ks"], "vs": r1[c]["vs"], "ozs": r1[c]["ozs"], "gs": r1[c]["gs"],
                    "cloc": r1[c]["cloc"], "nloc": r1[c]["nloc"], "gtot": r1[c]["gtot"],
                    "call": np.ascontiguousarray(np.stack([g["cloc"] for g in grp])),
                    "nall": np.ascontiguousarray(np.stack([g["nloc"] for g in grp])),
                    "gtall": np.ascontiguousarray(np.stack([g["gtot"] for g in grp], axis=1)),
                    "cmask": cmask, "cvalid": cvalid, "w_out1": w_out1,
                    "ident": ident, "tri": tri, "hng": hng, "postg": postg})
    nc2 = _get_nc(2)
    r2 = run_bass_kernel_spmd(nc2, in2, core_ids=list(range(8))).results
    out = np.empty((2, 4 * NSEG, D), np.float32)
    for c in range(8):
        b, j = c // 4, c % 4
        out[b, j * NSEG:(j + 1) * NSEG] = r2[c]["out"]
    return out
```
